# Optimizing a Trainium2 kernel written in Bass

```python
import jax, jax.numpy as jnp
from jax import lax
import numpy as np

D_MODEL = 2048
BATCH = 16
SEQ = 2048
DEPTH = 4

CHUNK = 64
W_A = D_MODEL // 2
W_B = D_MODEL // 2
W_C = D_MODEL // 2
CONV_A = 3
CONV_B = 31
SGU_BLOCK = 128
SGU_HEADS = 8
SGU_HEAD_DIM = W_C // SGU_HEADS
N_BRANCH = 3
IN_SPLITS = [W_A, 2 * W_A, 3 * W_A, 3 * W_A + W_B, 3 * W_A + 2 * W_B, 3 * W_A + 2 * W_B + W_C]
IN_COLS = 3 * W_A + 2 * W_B + 2 * W_C
N_EXPERTS = 32
TOP_K = 4
D_EXPERT = 3 * D_MODEL // 8
SWIGLU_LIMIT = 7.0
SWIGLU_ALPHA = 1.702
ROUTE_BLOCK = 128
N_MOD = 6
LN_EPS = 1e-5
DEEPNORM_ALPHA = (2.0 * DEPTH) ** 0.25
DEEPNORM_BETA = (8.0 * DEPTH) ** -0.25

kernel_name = "hybrid_conv_sgu_moe_streaming_encoder"


def layer_norm(x, gain, bias):
    xf = x.astype(jnp.float32)
    mu = jnp.mean(xf, axis=-1, keepdims=True)
    var = jnp.mean(jnp.square(xf - mu), axis=-1, keepdims=True)
    y = (xf - mu) * lax.rsqrt(var + LN_EPS) * gain.astype(jnp.float32) + bias.astype(jnp.float32)
    return y.astype(x.dtype)


def causal_depthwise_conv(x, w):
    k = w.shape[0]
    return lax.conv_general_dilated(
        x, w[:, None, :].astype(x.dtype), window_strides=(1,), padding=[(k - 1, 0)],
        dimension_numbers=("NWC", "WIO", "NWC"), feature_group_count=x.shape[-1])


def short_conv_mixer(b_gate, c_gate, hv, conv_w, w_out):
    y = c_gate * causal_depthwise_conv(b_gate * hv, conv_w)
    return y @ w_out


def conformer_conv(a, g, dw_w, dw_b, ln_g, ln_b, w_pw):
    y = a * jax.nn.sigmoid(g)
    y = causal_depthwise_conv(y, dw_w) + dw_b
    y = jax.nn.silu(layer_norm(y, ln_g, ln_b))
    return y @ w_pw


def sgu_mixer(u, v, ln_g, ln_b, w_s, b_s, w_out):
    u = jax.nn.gelu(u)
    v = layer_norm(jax.nn.gelu(v), ln_g, ln_b)
    bsz, seq, _ = v.shape
    nb = seq // SGU_BLOCK
    v = v.reshape(bsz, nb, SGU_BLOCK, SGU_HEADS, SGU_HEAD_DIM)
    chunk_id = jnp.arange(SGU_BLOCK) // CHUNK
    mask = chunk_id[:, None] >= chunk_id[None, :]
    w = jnp.where(mask[None], w_s, 0).astype(v.dtype)
    s = jnp.einsum("gij,bnjgd->bnigd", w, v) + b_s.T[:, :, None].astype(v.dtype)
    return (u * s.reshape(bsz, seq, W_C)) @ w_out


def moe_ffn(h, w_router, b_router, w_gu, b_gu, w_down, b_down):
    bsz, seq, d = h.shape
    xt = h.reshape(-1, d)
    n = xt.shape[0]
    nk = n * TOP_K
    logits = (xt @ w_router + b_router).astype(jnp.float32)
    top_logit, top_idx = lax.top_k(logits, TOP_K)
    top_w = jax.nn.softmax(top_logit, axis=-1)
    flat_e = top_idx.reshape(-1)
    order = jnp.argsort(flat_e)
    sorted_e = flat_e[order]
    counts = jnp.bincount(flat_e, length=N_EXPERTS)
    padded = (counts + ROUTE_BLOCK - 1) // ROUTE_BLOCK * ROUTE_BLOCK
    start = jnp.cumsum(counts) - counts
    pend = jnp.cumsum(padded)
    pstart = pend - padded
    dest = pstart[sorted_e] + jnp.arange(nk) - start[sorted_e]
    n_rows = (nk + ROUTE_BLOCK - 1) // ROUTE_BLOCK * ROUTE_BLOCK + N_EXPERTS * ROUTE_BLOCK
    n_blocks = n_rows // ROUTE_BLOCK
    tok_sorted = (order // TOP_K).astype(jnp.int32)
    row_token = jnp.full((n_rows,), n, jnp.int32).at[dest].set(tok_sorted)
    row_w = jnp.zeros((n_rows,), h.dtype).at[dest].set(top_w.reshape(-1)[order].astype(h.dtype))
    block_e = jnp.minimum(
        jnp.searchsorted(pend, jnp.arange(n_blocks) * ROUTE_BLOCK, side="right"), N_EXPERTS - 1)
    xb = xt[jnp.minimum(row_token, n - 1)].reshape(n_blocks, ROUTE_BLOCK, d)

    def expert_block(args):
        xblk, e = args
        gu = xblk @ w_gu[e] + b_gu[e]
        gate = jnp.minimum(gu[:, :D_EXPERT], SWIGLU_LIMIT)
        up = jnp.clip(gu[:, D_EXPERT:], -SWIGLU_LIMIT, SWIGLU_LIMIT)
        act = (up + 1) * (gate * jax.nn.sigmoid(SWIGLU_ALPHA * gate))
        return act @ w_down[e] + b_down[e]

    yb = lax.map(expert_block, (xb, block_e)).reshape(n_rows, d)
    y = jax.ops.segment_sum(yb * row_w[:, None], row_token, num_segments=n + 1)[:n]
    return y.reshape(bsz, seq, d)


def setup_inputs(seed: int = 0) -> dict:
    key = jax.random.key(seed)
    ks = jax.random.split(key, 32)
    L, D = DEPTH, D_MODEL

    def nrm(k, shape, s):
        return jax.random.normal(k, shape, jnp.float32) * s

    gate_offset = jnp.array([0.0, 0.0, 1.0, 0.0, 0.0, 1.0], jnp.float32)[None, :, None]
    return {
        "x": nrm(ks[0], (BATCH, SEQ, D), 1.0),
        "c": nrm(ks[1], (BATCH, D), 1.0),
        "w_ada": nrm(ks[2], (D, N_MOD * D), 0.3 * D ** -0.5),
        "b_ada": nrm(ks[3], (N_MOD * D,), 0.01),
        "ada_table": nrm(ks[4], (L, N_MOD, D), 0.1) + gate_offset,
        "w_in": nrm(ks[5], (L, D, IN_COLS), D ** -0.5),
        "conv_a": nrm(ks[6], (L, CONV_A, W_A), CONV_A ** -0.5),
        "dw_b_w": nrm(ks[7], (L, CONV_B, W_B), CONV_B ** -0.5),
        "dw_b_b": nrm(ks[8], (L, W_B), 0.01),
        "ln_b_g": 1.0 + nrm(ks[9], (L, W_B), 0.02),
        "ln_b_b": nrm(ks[10], (L, W_B), 0.01),
        "ln_c_g": 1.0 + nrm(ks[11], (L, W_C), 0.02),
        "ln_c_b": nrm(ks[12], (L, W_C), 0.01),
        "sgu_w": nrm(ks[13], (L, SGU_HEADS, SGU_BLOCK, SGU_BLOCK), SGU_BLOCK ** -0.5),
        "sgu_b": 1.0 + nrm(ks[14], (L, SGU_HEADS, SGU_BLOCK), 0.1),
        "w_out_a": nrm(ks[15], (L, W_A, D), DEEPNORM_BETA * W_A ** -0.5),
        "w_out_b": nrm(ks[16], (L, W_B, D), DEEPNORM_BETA * W_B ** -0.5),
        "w_out_c": nrm(ks[17], (L, W_C, D), DEEPNORM_BETA * W_C ** -0.5),
        "w_gate": nrm(ks[18], (L, D, N_BRANCH * D), D ** -0.5),
        "b_gate": nrm(ks[19], (L, N_BRANCH * D), 0.01),
        "w_o": nrm(ks[20], (L, D, D), DEEPNORM_BETA * D ** -0.5),
        "ln1_g": 1.0 + nrm(ks[21], (L, D), 0.02),
        "ln1_b": nrm(ks[22], (L, D), 0.01),
        "w_router": nrm(ks[23], (L, D, N_EXPERTS), D ** -0.5),
        "b_router": nrm(ks[24], (L, N_EXPERTS), 0.01),
        "w_gu": nrm(ks[25], (L, N_EXPERTS, D, 2 * D_EXPERT), D ** -0.5),
        "b_gu": nrm(ks[26], (L, N_EXPERTS, 2 * D_EXPERT), 0.01),
        "w_down": nrm(ks[27], (L, N_EXPERTS, D_EXPERT, D), DEEPNORM_BETA * D_EXPERT ** -0.5),
        "b_down": nrm(ks[28], (L, N_EXPERTS, D), 0.01),
        "ln2_g": 1.0 + nrm(ks[29], (L, D), 0.02),
        "ln2_b": nrm(ks[30], (L, D), 0.01),
    }


def reference(x, c, w_ada, b_ada, ada_table, w_in, conv_a, dw_b_w, dw_b_b, ln_b_g, ln_b_b,
              ln_c_g, ln_c_b, sgu_w, sgu_b, w_out_a, w_out_b, w_out_c, w_gate, b_gate, w_o,
              ln1_g, ln1_b, w_router, b_router, w_gu, b_gu, w_down, b_down, ln2_g, ln2_b):
    bsz, seq, d = x.shape
    mod_shared = (jax.nn.silu(c) @ w_ada + b_ada).reshape(bsz, N_MOD, d)
    for l in range(DEPTH):
        mod = mod_shared + ada_table[l]
        sh1, sc1, g1 = mod[:, 0, None, :], mod[:, 1, None, :], mod[:, 2, None, :]
        sh2, sc2, g2 = mod[:, 3, None, :], mod[:, 4, None, :], mod[:, 5, None, :]

        h = x * (1 + sc1) + sh1
        proj = h @ w_in[l]
        b_a, c_a, h_a, a_b, g_b, u_c, v_c = jnp.split(proj, IN_SPLITS, axis=-1)
        y_a = short_conv_mixer(b_a, c_a, h_a, conv_a[l], w_out_a[l])
        y_b = conformer_conv(a_b, g_b, dw_b_w[l], dw_b_b[l], ln_b_g[l], ln_b_b[l], w_out_b[l])
        y_c = sgu_mixer(u_c, v_c, ln_c_g[l], ln_c_b[l], sgu_w[l], sgu_b[l], w_out_c[l])
        gates = jax.nn.sigmoid(h @ w_gate[l] + b_gate[l]).reshape(bsz, seq, N_BRANCH, d)
        merged = gates[:, :, 0] * y_a + gates[:, :, 1] * y_b + gates[:, :, 2] * y_c
        x = layer_norm(DEEPNORM_ALPHA * x + g1 * (merged @ w_o[l]), ln1_g[l], ln1_b[l])

        h = x * (1 + sc2) + sh2
        y = moe_ffn(h, w_router[l], b_router[l], w_gu[l], b_gu[l], w_down[l], b_down[l])
        x = layer_norm(DEEPNORM_ALPHA * x + g2 * y, ln2_g[l], ln2_b[l])
    return x
```

```python
import numpy as np
from contextlib import ExitStack
import concourse.bass as bass
import concourse.mybir as mybir
from concourse.bass_utils import run_bass_kernel_spmd

F32 = mybir.dt.float32
BF16 = mybir.dt.bfloat16
I32 = mybir.dt.int32
AF = mybir.ActivationFunctionType
ALU = mybir.AluOpType
AX = mybir.AxisListType

D = 2048
KT = 16
WA = 1024
NE = 32
DE = 768
LN_EPS = 1e-5
NCORES = 8


class Cfg:
    def __init__(self, L=4, NB=2, S=2048, CAP=1280, dbg=False, cnt=True):
        self.L, self.NB, self.S, self.CAP, self.dbg, self.cnt = L, NB, S, CAP, dbg, cnt
        self.NTOK = NB * S
        self.NT = self.NTOK // 128
        self.NROWS = NE * CAP
        self.alpha = (2.0 * 4) ** 0.25


class Sem:
    def __init__(self, handle, is_dma):
        self.h, self.is_dma, self.total = handle, is_dma, 0


class Buf:
    def __init__(self, name):
        self.name = name
        self.w = {}
        self.r = {}


class Tracker:
    ENG = ["pe", "act", "dve", "pool", "sp"]

    def __init__(self, nc, es):
        self.nc, self.es = nc, es
        self.ops = {e: [] for e in self.ENG}
        self.esem = {}
        for e in self.ENG:
            self.esem[e] = Sem(es.enter_context(nc.semaphore("s_" + e)), False)
        self.waited = {e: {} for e in self.ENG}
        self.dsems = []
        self.nsem = 0
        self.init = {}

    def dma_sem(self, name=None):
        self.nsem += 1
        s = Sem(self.es.enter_context(self.nc.semaphore("d%d" % self.nsem)), True)
        self.dsems.append(s)
        return s

    def _waits(self, eng, deps):
        out = []
        for s, v in deps.items():
            if s.is_dma:
                v = s.total
            elif s is self.esem[eng] and eng in ("pe", "pool"):
                continue
            if v <= 0:
                continue
            if self.waited[eng].get(s, 0) >= v:
                continue
            self.waited[eng][s] = v
            out.append((s.h, v))
        return out

    def op(self, eng, fn, reads=(), writes=(), dsem=None, inc=None):
        deps = {}
        for b in reads:
            for s, v in b.w.items():
                deps[s] = max(deps.get(s, 0), v)
        for b in writes:
            for s, v in list(b.w.items()) + list(b.r.items()):
                deps[s] = max(deps.get(s, 0), v)
        waits = self._waits(eng, deps)
        if dsem is not None:
            dsem.total += (16 if inc is None else inc)
            s, v, amt = dsem, dsem.total, (16 if inc is None else inc)
        else:
            s = self.esem[eng]
            s.total += 1
            v, amt = s.total, 1
        self.ops[eng].append((waits, fn, s.h, amt))
        for b in reads:
            b.r[s] = max(b.r.get(s, 0), v)
        for b in writes:
            b.w[s] = max(b.w.get(s, 0), v)

    def barrier(self):
        allsems = list(self.esem.values()) + self.dsems
        for e in self.ENG:
            waits = []
            for s in allsems:
                if s is self.esem[e] or s.total == 0:
                    continue
                if self.waited[e].get(s, 0) >= s.total:
                    continue
                self.waited[e][s] = s.total
                waits.append((s.h, s.total))
            if waits:
                self.ops[e].append((waits, None, None, 0))

    def emit(self):
        nc = self.nc
        final = [(s.h, s.total) for s in list(self.esem.values()) + self.dsems if s.total > 0]
        with nc.Block() as block:
            def run(eng_name, last=False):
                def body(e):
                    if eng_name in self.init:
                        self.init[eng_name](e)
                    for waits, fn, sh, amt in self.ops[eng_name]:
                        for h, v in waits:
                            e.wait_ge(h, v)
                        if fn is not None:
                            fn(e).then_inc(sh, amt)
                    if last:
                        for h, v in final:
                            e.wait_ge(h, v)
                return body
            block.tensor(run("pe"))
            block.scalar(run("act"))
            block.vector(run("dve"))
            block.gpsimd(run("pool"))
            block.sync(run("sp", last=True))


WFAM = [
    ("w_in", 256, 7168), ("w_gate", 256, 6144), ("w_o", 256, 2048),
    ("w_oa", 128, 2048), ("w_ob", 128, 2048), ("w_oc", 128, 2048),
    ("w_gu", 4 * 2048, 1536), ("w_dn", 4 * 768, 2048),
]

PP = {}
_off = 0
for _n, _w in [("conv_a", 8 * 3), ("dw_w", 8 * 31), ("dw_b", 8), ("lnb_g", 8), ("lnb_b", 8),
               ("b_gate", 48), ("b_gu", 32 * 12)]:
    PP[_n] = (_off, _w)
    _off += _w
PPW = _off


def build(cfg):
    L, NB, S, CAP, NTOK, NT, NROWS = cfg.L, cfg.NB, cfg.S, cfg.CAP, cfg.NTOK, cfg.NT, cfg.NROWS
    TC = 512
    NCH = NTOK // TC
    CPS = S // TC
    nc = bass.Bass("TRN2", target_bir_lowering=False)
    es = ExitStack()
    T = Tracker(nc, es)

    def din(name, shape, dt=F32):
        return nc.dram_tensor(name, list(shape), dt, kind="ExternalInput").ap()

    x_in = din("x", [NTOK, D])
    cT_in = din("cT", [128, KT, 16])
    wada_in = din("w_ada", [128, KT, 1536])
    bada_in = din("b_ada", [128, 12])
    adaT_in = din("adaT", [128, L * 6 * KT])
    wsh = {n: din(n + "_s", [L, r, c]) for n, r, c in WFAM}
    pp_in = din("pp", [128, L * PPW])
    rows_in = din("rows", [L, 6, D])
    sguw_in = din("sgu_w", [L, 8, 128, 128])
    sgub_in = din("sgu_b", [L, 8 * 128])
    wr_in = din("w_router", [L, 128, KT, NE])
    br_in = din("b_router", [L, NE])
    bdn_in = din("b_down", [L, NE, D])
    ident_in = din("ident", [128, 128])
    bsel_in = din("bsel", [128, NB * 16])
    triu_in = din("triu", [128, 128])
    sel_in = din("sel", [NE, NE * 128])
    erow_in = din("erow", [1, NE])
    y_out = nc.dram_tensor("y", [NTOK, D], F32, kind="ExternalOutput").ap()
    if cfg.cnt:
        cnt_out = nc.dram_tensor("cnt", [L, NE], F32, kind="ExternalOutput").ap()
        B_cnt = Buf("cnt")
    if cfg.dbg:
        dbg_out = nc.dram_tensor("dbg", [4, D, 512], BF16, kind="ExternalOutput").ap()
        B_dbg = Buf("dbg")

    wbf_s = {(n, l): nc.dram_tensor("%s_b%d" % (n, l), [r, c], BF16) for n, r, c in WFAM for l in range(L)}
    wfull = {(n, l): nc.dram_tensor("%s_f%d" % (n, l), [8 * r, c], BF16) for n, r, c in WFAM for l in range(L)}
    cc_in = nc.dram_tensor("cc_in", [12 * 128, 16], F32)
    cc_out = nc.dram_tensor("cc_out", [96 * 128, 16], F32)
    xres = nc.dram_tensor("xres", [NTOK, D], F32).ap()
    xb = nc.dram_tensor("xb", [NROWS + 128, D], BF16).ap()
    ybh = [nc.dram_tensor("yb%d" % i, [NROWS + 128, D // 2], F32).ap() for i in range(2)]
    B_xres, B_xb, B_yb, B_y = Buf("xres"), Buf("xb"), Buf("yb"), Buf("y")
    B_wfull = {k: Buf("wf") for k in wfull}

    ARENA_W = 52000
    arena = es.enter_context(nc.sbuf_tensor("arena", [128, ARENA_W], F32))
    apos = [0]

    def carve(shape, dt, name="t"):
        n = int(np.prod(shape))
        nbytes = n * (4 if dt in (F32, I32) else 2)
        words = (nbytes + 3) // 4
        words = (words + 7) // 8 * 8
        off = apos[0]
        apos[0] += words
        assert apos[0] <= ARENA_W, "SBUF arena overflow at %s: %d" % (name, apos[0])
        v = arena[:, off:off + words]
        if dt != F32:
            v = v.bitcast(dt)
        v = v[:, 0:n]
        if len(shape) == 2:
            v = v.rearrange("p (a b) -> p a b", a=shape[0])
        elif len(shape) == 3:
            v = v.rearrange("p (a b c) -> p a b c", a=shape[0], b=shape[1])
        return v, Buf(name)

    psum = []
    for i in range(8):
        t = es.enter_context(nc.psum_tensor("ps%d" % i, [128, 512], F32))
        psum.append((t[:, :], Buf("ps%d" % i)))
    pctr = [0]

    def next_ps():
        i = pctr[0] % 8
        pctr[0] += 1
        return psum[i]

    ident_f, B_identf = carve([128], F32, "identf")
    ident_b, B_identb = carve([128], BF16, "identb")
    triu_b, B_triu = carve([128], BF16, "triu")
    ones_b, B_onesb = carve([128], BF16, "onesb")
    ones_f, B_onesf = carve([128], F32, "onesf")
    selt, B_selt = carve([128], BF16, "selt")
    erow, B_erow = carve([NE], F32, "erow")
    epsc, B_epsc = carve([8], F32, "epsc")
    adaT, B_adaT = carve([L * 6 * KT], F32, "adaT")
    modc, B_modc = carve([L * NB * 6, KT], F32, "modc")
    pp, B_pp = carve([PPW], F32, "pp")
    ridx, B_ridx = carve([NT, 4], I32, "ridx")
    rw, B_rw = carve([NT, 4], F32, "rw")
    persist_end = apos[0]

    dq = [0]

    SEMS = {}

    def nsem(name):
        if name not in SEMS:
            SEMS[name] = T.dma_sem()
        return SEMS[name]

    def dma(eng, out, in_, reads, writes, sem):
        T.op(eng, lambda e: e.dma_start(out=out, in_=in_), reads=reads, writes=writes, dsem=sem)

    s_const = T.dma_sem()
    for dst, bdst, src in [(ident_f, B_identf, ident_in), (adaT, B_adaT, adaT_in)]:
        dma("sp", dst, src, [], [bdst], s_const)
    dma("sp", erow, erow_in.partition_broadcast(128), [], [B_erow], s_const)
    dma("pool", ident_b, ident_in, [], [B_identb], s_const)
    dma("pool", triu_b, triu_in, [], [B_triu], s_const)
    T.op("dve", lambda e: e.memset(ones_b, 1.0), writes=[B_onesb])
    T.op("dve", lambda e: e.memset(ones_f, 1.0), writes=[B_onesf])
    T.op("dve", lambda e: e.memset(epsc, LN_EPS), writes=[B_epsc])

    s_cast = [T.dma_sem() for _ in range(L)]
    s_ag = [T.dma_sem() for _ in range(L)]
    B_wbf = {k: Buf("wbf") for k in wbf_s}
    MAXEL = 128 * 8192
    rg = [list(range(NCORES))]
    for l in range(L):
        for n, r, c in WFAM:
            tot = r * c
            src = wsh[n][l].rearrange("r c -> (r c)")
            dst = wbf_s[(n, l)].ap().rearrange("r c -> (r c)")
            o = 0
            while o < tot:
                m = min(MAXEL, tot - o)
                assert m % 128 == 0
                dma("pool", dst[o:o + m].rearrange("(p f) -> p f", p=128),
                    src[o:o + m].rearrange("(p f) -> p f", p=128), [], [B_wbf[(n, l)]], s_cast[l])
                o += m
        for n, r, c in WFAM:
            i_ap, o_ap = wbf_s[(n, l)].ap().opt(), wfull[(n, l)].ap().opt()
            T.op("pool", lambda e, i_ap=i_ap, o_ap=o_ap: e.collective_compute(
                "AllGather", ALU.bypass, replica_groups=rg, ins=[i_ap], outs=[o_ap]),
                reads=[B_wbf[(n, l)]], writes=[B_wfull[(n, l)]], dsem=s_ag[l], inc=1)

    mark = apos[0]
    modT, B_modT = carve([96, 16], F32, "modT")
    cT, B_cT = carve([KT, 16], F32, "cT")
    bsel, B_bsel = carve([NB * 16], F32, "bsel")
    msel, B_msel = carve([NB, 96], F32, "msel")
    wada, B_wada = carve([KT, 1536], F32, "wada")
    bada, B_bada = carve([12], F32, "bada")
    mout, B_mout = carve([12, 16], F32, "mout")
    s_pro = T.dma_sem()
    dma("sp", cT, cT_in, [], [B_cT], s_pro)
    dma("sp", wada, wada_in, [], [B_wada], s_pro)
    dma("sp", bada, bada_in, [], [B_bada], s_pro)
    dma("sp", bsel, bsel_in, [], [B_bsel], s_pro)
    T.op("act", lambda e: e.activation(out=cT, in_=cT, func=AF.Silu), reads=[B_cT], writes=[B_cT])
    for nt in range(12):
        pt, pb = next_ps()
        for kt in range(KT):
            T.op("pe", lambda e, pt=pt, nt=nt, kt=kt: e.matmul(
                pt[:, 0:16], lhsT=wada[:, kt, nt * 128:(nt + 1) * 128], rhs=cT[:, kt, :],
                start=(kt == 0), stop=(kt == KT - 1)), reads=[B_wada, B_cT], writes=[pb])
        T.op("dve", lambda e, pt=pt, nt=nt: e.tensor_scalar(
            out=mout[:, nt, :], in0=pt[:, 0:16], scalar1=bada[:, nt:nt + 1], scalar2=None, op0=ALU.add),
            reads=[pb, B_bada], writes=[B_mout])
    B_ccin, B_ccout = Buf("ccin"), Buf("ccout")
    s_cc = T.dma_sem()
    dma("sp", cc_in.ap().rearrange("(nt p) b -> p nt b", p=128), mout, [B_mout], [B_ccin], s_pro)
    T.op("pool", lambda e: e.collective_compute(
        "AllGather", ALU.bypass, replica_groups=rg, ins=[cc_in.ap().opt()], outs=[cc_out.ap().opt()]),
        reads=[B_ccin], writes=[B_ccout], dsem=s_cc, inc=1)
    dma("sp", modT, cc_out.ap().rearrange("(nt p) b -> p nt b", p=128), [B_ccout], [B_modT], s_pro)

    for bl in range(NB):
        for b in range(16):
            if b == 0:
                T.op("dve", lambda e, bl=bl, b=b: e.tensor_scalar(
                    out=msel[:, bl, :], in0=modT[:, :, b], scalar1=bsel[:, bl * 16 + b: bl * 16 + b + 1], scalar2=None,
                    op0=ALU.mult), reads=[B_modT, B_bsel], writes=[B_msel])
            else:
                T.op("dve", lambda e, bl=bl, b=b: e.scalar_tensor_tensor(
                    out=msel[:, bl, :], in0=modT[:, :, b], scalar=bsel[:, bl * 16 + b: bl * 16 + b + 1], in1=msel[:, bl, :],
                    op0=ALU.mult, op1=ALU.add), reads=[B_modT, B_bsel, B_msel], writes=[B_msel])
    for l in range(L):
        for bl in range(NB):
            for m in range(6):
                j = (l * NB + bl) * 6 + m
                T.op("dve", lambda e, j=j, m=m, bl=bl, l=l: e.tensor_tensor(
                    out=modc[:, j, :], in0=msel[:, bl, m * 16:(m + 1) * 16],
                    in1=adaT[:, (l * 6 + m) * 16:(l * 6 + m + 1) * 16], op=ALU.add),
                    reads=[B_msel, B_adaT], writes=[B_modc])
                if m in (1, 4):
                    T.op("dve", lambda e, j=j: e.tensor_scalar_add(modc[:, j, :], modc[:, j, :], 1.0),
                         reads=[B_modc], writes=[B_modc])

    T.barrier()
    apos[0] = mark
    mark = apos[0]
    zt, B_zt = carve([8192], BF16, "zt")
    T.op("dve", lambda e: e.memset(zt, 0.0), writes=[B_zt])
    s_z = T.dma_sem()
    xbf = xb.rearrange("r c -> (r c)")
    o, tot = 0, (NROWS + 128) * D
    while o < tot:
        m = min(128 * 8192, tot - o)
        dma("sp", xbf[o:o + m].rearrange("(p f) -> p f", p=128), zt[:, 0:m // 128], [B_zt], [B_xb], s_z)
        o += m
    for hh in range(2):
        dma("sp", ybh[hh][NROWS:NROWS + 128, :], zt.bitcast(F32)[:, 0:D // 2], [B_zt], [B_yb], s_z)
    T.barrier()
    apos[0] = mark

    def ppv(l, name, j=0, w=1):
        o, _ = PP[name]
        return pp[:, o + j: o + j + w]

    def mcol(l, bl, m, kt):
        j = (l * NB + bl) * 6 + m
        return modc[:, j, kt:kt + 1]

    def grow_build(l, bl, m, grow, B_grow, tmp, B_tmp):
        for q in range(4):
            pt, pb = next_ps()
            for i in range(4):
                kt = q * 4 + i
                T.op("dve", lambda e, kt=kt: e.tensor_scalar(
                    out=tmp, in0=ones_f, scalar1=mcol(l, bl, m, kt), scalar2=None, op0=ALU.mult),
                    reads=[B_onesf, B_modc], writes=[B_tmp])
                T.op("pe", lambda e, pt=pt, i=i: e.matmul(
                    pt[:, i * 128:(i + 1) * 128], lhsT=tmp, rhs=ident_f, start=True, stop=True),
                    reads=[B_tmp, B_identf], writes=[pb])
            T.op("act", lambda e, pt=pt, q=q: e.copy(out=grow[:, q * 512:(q + 1) * 512], in_=pt),
                 reads=[pb], writes=[B_grow])

    def rstd_op(out, var, b_out, b_var):
        T.op("act", lambda e: e.activation(out=out, in_=var, func=AF.Sqrt, bias=epsc[:, 0:1], scale=1.0),
             reads=[b_var, B_epsc], writes=[b_out])
        T.op("dve", lambda e: e.reciprocal(out=out, in_=out), reads=[b_out], writes=[b_out])

    def layer_norm_rows(xt, B_xt, grow_g, grow_b, B_rows, st, B_st):
        for q in range(4):
            T.op("dve", lambda e, q=q: e.bn_stats(out=st[:, q * 6:(q + 1) * 6], in_=xt[:, q * 512:(q + 1) * 512]),
                 reads=[B_xt], writes=[B_st])
        T.op("dve", lambda e: e.bn_aggr(out=st[:, 24:26], in_=st[:, 0:24]), reads=[B_st], writes=[B_st])
        rstd_op(st[:, 26:27], st[:, 25:26], B_st, B_st)
        T.op("dve", lambda e: e.tensor_scalar(out=xt, in0=xt, scalar1=st[:, 24:25], scalar2=st[:, 26:27],
                                              op0=ALU.subtract, op1=ALU.mult), reads=[B_xt, B_st], writes=[B_xt])
        T.op("pool", lambda e: e.tensor_tensor(out=xt, in0=xt, in1=grow_g, op=ALU.mult),
             reads=[B_xt, B_rows], writes=[B_xt])
        T.op("pool", lambda e: e.tensor_tensor(out=xt, in0=xt, in1=grow_b, op=ALU.add),
             reads=[B_xt, B_rows], writes=[B_xt])

    def mixer(l, xsrc, B_xsrc, xdst, B_xdst):
        apos[0] = persist_end
        xt, B_xt = carve([D], F32, "xt")
        rs, B_rs = carve([D], F32, "rs")
        g1row, B_g1 = carve([D], F32, "g1row")
        lnr, B_lnr = carve([2, D], F32, "lnr")
        lncr, B_lncr = carve([2, WA], F32, "lncr")
        hT, B_hT = carve([KT, TC], BF16, "hT")
        NWS = 3
        wsl = [carve([KT, 256], BF16, "wsl%d" % i) for i in range(NWS)]
        s_w = [nsem("mxw%d" % _i) for _i in range(NWS)]
        zbuf, B_zbuf = carve([8, 2 + TC], BF16, "zbuf")
        ya, B_ya = carve([8, TC], BF16, "ya")
        ybuf, B_ybuf = carve([8, 30 + TC], BF16, "ybuf")
        cv, B_cv = carve([8, TC], BF16, "cv")
        diag, B_diag = carve([31, 128], BF16, "diag")
        ug, B_ug = carve([8, TC], BF16, "ug")
        vln, B_vln = carve([4, WA], BF16, "vln")
        tA = [carve([TC], F32, "tA%d" % i) for i in range(4)]
        tV = [carve([WA], F32, "tV%d" % i) for i in range(2)]
        merged, B_mg = carve([KT, TC], BF16, "merged")
        sguT, B_sguT = carve([8, 128], BF16, "sguT")
        sgub, B_sgub = carve([8 * 128], F32, "sgub")
        st, B_st = carve([32], F32, "st")
        tmp128, B_tmp128 = carve([128], F32, "tmp128")
        s_x, s_o, s_p = nsem("k1"), nsem("k2"), nsem("k3")
        sguw, B_sguw = tV[0][0].rearrange("p (g j) -> p g j", g=8), tV[0][1]

        dma("sp", pp, pp_in[:, l * PPW:(l + 1) * PPW], [], [B_pp], s_p)
        dma("sp", lnr[:, 0, :], rows_in[l, 0:1, :].partition_broadcast(128), [], [B_lnr], s_p)
        dma("sp", lnr[:, 1, :], rows_in[l, 1:2, :].partition_broadcast(128), [], [B_lnr], s_p)
        dma("sp", lncr[:, 0, :], rows_in[l, 4:5, 0:WA].partition_broadcast(128), [], [B_lncr], s_p)
        dma("sp", lncr[:, 1, :], rows_in[l, 4:5, WA:D].partition_broadcast(128), [], [B_lncr], s_p)
        dma("sp", sgub, sgub_in[l:l + 1, :].partition_broadcast(128), [], [B_sgub], s_p)
        dma("sp", sguw, sguw_in[l].rearrange("g i j -> i g j"), [], [B_sguw], s_p)
        for g in range(8):
            pt, pb = next_ps()
            T.op("pe", lambda e, pt=pt, g=g: e.matmul(pt[:, 0:128], lhsT=sguw[:, g, :], rhs=ident_f,
                                                        start=True, stop=True),
                 reads=[B_sguw, B_identf], writes=[pb])
            T.op("act", lambda e, pt=pt, g=g: e.copy(out=sguT[:, g, :], in_=pt[:, 0:128]),
                 reads=[pb], writes=[B_sguT])
        T.op("dve", lambda e: e.memset(sguT[64:128, :, 0:64], 0.0), reads=[B_sguT], writes=[B_sguT])

        wq = [0]

        def wload(name, rows_k, c0, ncols):
            i = wq[0] % NWS
            wq[0] += 1
            (w, bw) = wsl[i]
            src = wfull[(name, l)].ap()[:, c0:c0 + ncols].rearrange("(kt p) c -> p kt c", p=128)
            dma("sp", w[:, 0:rows_k, 0:ncols], src, [B_wfull[(name, l)]], [bw], s_w[i])
            return w, bw

        def evac_act(out, ob, pt, pb, func=AF.Copy, **kw):
            T.op("act", lambda e: e.activation(out=out, in_=pt, func=func, **kw), reads=[pb] + kw.pop("_r", []),
                 writes=[ob])

        for ch in range(NCH):
            bl = ch // CPS
            first = (ch % CPS == 0)
            t0 = ch * TC
            if first:
                grow_build(l, bl, 2, g1row, B_g1, tmp128, B_tmp128)
            for tt in range(4):
                r0 = t0 + tt * 128
                dma("act", xt, xsrc[r0:r0 + 128, :], [B_xsrc], [B_xt], s_x)
                for q in range(4):
                    pt, pb = next_ps()
                    for i in range(4):
                        kt = q * 4 + i
                        T.op("pe", lambda e, pt=pt, i=i, kt=kt: e.matmul(
                            pt[:, i * 128:(i + 1) * 128], lhsT=xt[:, kt * 128:(kt + 1) * 128], rhs=ident_f,
                            start=True, stop=True), reads=[B_xt, B_identf], writes=[pb])
                    for i in range(4):
                        kt = q * 4 + i
                        T.op("act", lambda e, pt=pt, i=i, kt=kt, tt=tt, bl=bl: e.activation(
                            out=hT[:, kt, tt * 128:(tt + 1) * 128], in_=pt[:, i * 128:(i + 1) * 128],
                            func=AF.Identity, bias=mcol(l, bl, 0, kt), scale=mcol(l, bl, 1, kt)),
                            reads=[pb, B_modc], writes=[B_hT])

            def proj(name, c0, K=KT, rhs=None, B_rhs=None):
                raise NotImplementedError

            def fm_tiles(name, col_tiles, rows_k, rhs, B_rhs):
                res = []
                i = 0
                while i < len(col_tiles):
                    c = col_tiles[i]
                    two = (i + 1 < len(col_tiles) and col_tiles[i + 1] == c + 1)
                    w, bw = wload(name, rows_k, c * 128, 256 if two else 128)
                    for j in range(2 if two else 1):
                        pt, pb = next_ps()
                        for kt in range(rows_k):
                            T.op("pe", lambda e, pt=pt, w=w, kt=kt, j=j: e.matmul(
                                pt, lhsT=w[:, kt, j * 128:(j + 1) * 128], rhs=rhs[:, kt, :],
                                start=(kt == 0), stop=(kt == rows_k - 1)), reads=[bw, B_rhs], writes=[pb])
                        res.append((pt, pb))
                    i += 2 if two else 1
                return res

            if first:
                T.op("dve", lambda e: e.memset(zbuf[:, :, 0:2], 0.0), writes=[B_zbuf])
                T.op("dve", lambda e: e.memset(ybuf[:, :, 0:30], 0.0), writes=[B_ybuf])
            else:
                T.op("dve", lambda e: e.tensor_copy(out=zbuf[:, :, 0:2], in_=zbuf[:, :, TC:TC + 2]),
                     reads=[B_zbuf], writes=[B_zbuf])
                T.op("dve", lambda e: e.tensor_copy(out=ybuf[:, :, 0:30], in_=ybuf[:, :, TC:TC + 30]),
                     reads=[B_ybuf], writes=[B_ybuf])
            for j in range(8):
                (pb_, bpb), (pc_, bpc), (ph_, bph) = fm_tiles("w_in", [j], KT, hT, B_hT) + \
                    fm_tiles("w_in", [8 + j], KT, hT, B_hT) + fm_tiles("w_in", [16 + j], KT, hT, B_hT)
                (t0_, bt0), (t1_, bt1) = tA[0], tA[1]
                T.op("act", lambda e, pb_=pb_, t0_=t0_: e.copy(out=t0_, in_=pb_), reads=[bpb], writes=[bt0])
                T.op("dve", lambda e, ph_=ph_, t0_=t0_, j=j: e.tensor_tensor(
                    out=zbuf[:, j, 2:2 + TC], in0=ph_, in1=t0_, op=ALU.mult), reads=[bph, bt0], writes=[B_zbuf])
                T.op("act", lambda e, pc_=pc_, t1_=t1_: e.copy(out=t1_, in_=pc_), reads=[bpc], writes=[bt1])
                for k in range(3):
                    T.op("dve", lambda e, k=k, j=j: e.tensor_scalar(
                        out=diag[:, k, :], in0=ident_b, scalar1=ppv(l, "conv_a", j * 3 + k), scalar2=None,
                        op0=ALU.mult), reads=[B_identb, B_pp], writes=[B_diag])
                pt, pb = next_ps()
                for k in range(3):
                    T.op("pe", lambda e, pt=pt, k=k, j=j: e.matmul(
                        pt, lhsT=diag[:, k, :], rhs=zbuf[:, j, k:k + TC], start=(k == 0), stop=(k == 2)),
                        reads=[B_diag, B_zbuf], writes=[pb])
                T.op("dve", lambda e, pt=pt, t1_=t1_, j=j: e.tensor_tensor(
                    out=ya[:, j, :], in0=pt, in1=t1_, op=ALU.mult), reads=[pb, bt1], writes=[B_ya])

            for j in range(8):
                (pa_, bpa), (pg_, bpg) = fm_tiles("w_in", [24 + j], KT, hT, B_hT) + \
                    fm_tiles("w_in", [32 + j], KT, hT, B_hT)
                (t0_, bt0) = tA[0]
                T.op("act", lambda e, pg_=pg_, t0_=t0_: e.activation(out=t0_, in_=pg_, func=AF.Sigmoid),
                     reads=[bpg], writes=[bt0])
                T.op("dve", lambda e, pa_=pa_, t0_=t0_, j=j: e.tensor_tensor(
                    out=ybuf[:, j, 30:30 + TC], in0=pa_, in1=t0_, op=ALU.mult), reads=[bpa, bt0], writes=[B_ybuf])
                for k in range(31):
                    T.op("dve", lambda e, k=k, j=j: e.tensor_scalar(
                        out=diag[:, k, :], in0=ident_b, scalar1=ppv(l, "dw_w", j * 31 + k), scalar2=None,
                        op0=ALU.mult), reads=[B_identb, B_pp], writes=[B_diag])
                pt, pb = next_ps()
                for k in range(31):
                    T.op("pe", lambda e, pt=pt, k=k, j=j: e.matmul(
                        pt, lhsT=diag[:, k, :], rhs=ybuf[:, j, k:k + TC], start=(k == 0), stop=(k == 30)),
                        reads=[B_diag, B_ybuf], writes=[pb])
                T.op("act", lambda e, pt=pt, j=j: e.activation(
                    out=cv[:, j, :], in_=pt, func=AF.Identity, bias=ppv(l, "dw_b", j), scale=1.0),
                    reads=[pb, B_pp], writes=[B_cv])
            psm, bsm = next_ps()
            for j in range(8):
                T.op("pe", lambda e, j=j, psm=psm: e.matmul(psm, lhsT=ones_b, rhs=cv[:, j, :], start=(j == 0), stop=(j == 7)),
                     reads=[B_onesb, B_cv], writes=[bsm])
            psq, bsq = next_ps()
            (sq_, bsq_) = tA[0]
            sqb = sq_.bitcast(BF16)[:, 0:TC]
            for j in range(8):
                T.op("dve", lambda e, j=j: e.tensor_tensor(out=sqb, in0=cv[:, j, :], in1=cv[:, j, :], op=ALU.mult),
                     reads=[B_cv], writes=[bsq_])
                T.op("pe", lambda e, j=j, psq=psq: e.matmul(psq, lhsT=ones_b, rhs=sqb, start=(j == 0), stop=(j == 7)),
                     reads=[B_onesb, bsq_], writes=[bsq])
            (mean_, bmean), (rstd_, brstd), (t2_, bt2) = tA[1], tA[2], tA[3]
            T.op("act", lambda e, psm=psm: e.activation(
                out=mean_, in_=psm, func=AF.Copy, scale=1.0 / WA), reads=[bsm], writes=[bmean])
            T.op("dve", lambda e: e.tensor_tensor(out=t2_, in0=mean_, in1=mean_, op=ALU.mult),
                 reads=[bmean], writes=[bt2])
            T.op("dve", lambda e, psq=psq: e.scalar_tensor_tensor(out=rstd_, in0=psq, scalar=1.0 / WA, in1=t2_,
                                                         op0=ALU.mult, op1=ALU.subtract),
                 reads=[bsq, bt2], writes=[brstd])
            rstd_op(rstd_, rstd_, brstd, brstd)
            for j in range(8):
                (t0_, bt0) = tA[0]
                T.op("dve", lambda e, j=j, t0_=t0_: e.tensor_tensor(out=t0_, in0=cv[:, j, :], in1=mean_, op=ALU.subtract),
                     reads=[B_cv, bmean], writes=[bt0])
                T.op("dve", lambda e, t0_=t0_: e.tensor_tensor(out=t0_, in0=t0_, in1=rstd_, op=ALU.mult),
                     reads=[bt0, brstd], writes=[bt0])
                T.op("act", lambda e, j=j, t0_=t0_: e.activation(
                    out=cv[:, j, :], in_=t0_, func=AF.Silu, bias=ppv(l, "lnb_b", j), scale=ppv(l, "lnb_g", j)),
                    reads=[bt0, B_pp], writes=[B_cv])

            def gelu_from(pt, pb, out, ob, ta, bta, tb, btb, n):
                T.op("act", lambda e: e.activation(out=ta[:, 0:n], in_=pt, func=AF.Square), reads=[pb], writes=[bta])
                T.op("dve", lambda e: e.tensor_scalar(out=ta[:, 0:n], in0=ta[:, 0:n], scalar1=0.044715, scalar2=1.0,
                                                      op0=ALU.mult, op1=ALU.add), reads=[bta], writes=[bta])
                T.op("dve", lambda e: e.tensor_tensor(out=ta[:, 0:n], in0=ta[:, 0:n], in1=pt, op=ALU.mult),
                     reads=[bta, pb], writes=[bta])
                T.op("act", lambda e: e.activation(out=ta[:, 0:n], in_=ta[:, 0:n], func=AF.Sigmoid,
                                                   scale=1.5957691216057308), reads=[bta], writes=[bta])
                T.op("dve", lambda e: e.tensor_tensor(out=out, in0=ta[:, 0:n], in1=pt, op=ALU.mult),
                     reads=[bta, pb], writes=[ob])

            for j in range(8):
                (pu_, bpu), = fm_tiles("w_in", [40 + j], KT, hT, B_hT)
                gelu_from(pu_, bpu, ug[:, j, :], B_ug, tA[0][0], tA[0][1], None, None, TC)
            for tt in range(4):
                (v_, bv) = tV[0]
                for cb in range(4):
                    w, bw = wload("w_in", KT, 6144 + cb * 256, 256)
                    if cb % 2 == 0:
                        pt, pb = next_ps()
                    for kt in range(KT):
                        T.op("pe", lambda e, pt=pt, w=w, kt=kt, cb=cb, tt=tt: e.matmul(
                            pt[:, (cb % 2) * 256:(cb % 2) * 256 + 256], lhsT=hT[:, kt, tt * 128:(tt + 1) * 128],
                            rhs=w[:, kt, 0:256], start=(kt == 0), stop=(kt == KT - 1)),
                            reads=[bw, B_hT], writes=[pb])
                    if cb % 2 == 1:
                        h0 = (cb // 2) * 512
                        gelu_from(pt, pb, v_[:, h0:h0 + 512], bv, tV[1][0][:, h0:h0 + 512], tV[1][1], None, None, 512)
                for q in range(2):
                    T.op("dve", lambda e, q=q: e.bn_stats(out=st[:, q * 6:(q + 1) * 6], in_=v_[:, q * 512:(q + 1) * 512]),
                         reads=[bv], writes=[B_st])
                T.op("dve", lambda e: e.bn_aggr(out=st[:, 24:26], in_=st[:, 0:12]), reads=[B_st], writes=[B_st])
                rstd_op(st[:, 26:27], st[:, 25:26], B_st, B_st)
                T.op("dve", lambda e: e.tensor_scalar(out=v_, in0=v_, scalar1=st[:, 24:25], scalar2=st[:, 26:27],
                                                      op0=ALU.subtract, op1=ALU.mult), reads=[bv, B_st], writes=[bv])
                T.op("pool", lambda e: e.tensor_tensor(out=v_, in0=v_, in1=lncr[:, 0, :], op=ALU.mult),
                     reads=[bv, B_lncr], writes=[bv])
                T.op("pool", lambda e, tt=tt: e.tensor_tensor(out=vln[:, tt, :], in0=v_, in1=lncr[:, 1, :], op=ALU.add),
                     reads=[bv, B_lncr], writes=[B_vln])
            for g in range(8):
                pt, pb = next_ps()
                for tt in range(4):
                    T.op("pe", lambda e, pt=pt, g=g, tt=tt: e.matmul(
                        pt[:, tt * 128:(tt + 1) * 128], lhsT=vln[:, tt, g * 128:(g + 1) * 128], rhs=sguT[:, g, :],
                        start=True, stop=True), reads=[B_vln, B_sguT], writes=[pb])
                (t0_, bt0) = tA[0]
                for tt in range(4):
                    T.op("dve", lambda e, pt=pt, g=g, tt=tt, t0_=t0_: e.tensor_tensor(
                        out=t0_[:, tt * 128:(tt + 1) * 128], in0=pt[:, tt * 128:(tt + 1) * 128],
                        in1=sgub[:, g * 128:(g + 1) * 128], op=ALU.add), reads=[pb, B_sgub], writes=[bt0])
                T.op("dve", lambda e, g=g, t0_=t0_: e.tensor_tensor(out=ug[:, g, :], in0=ug[:, g, :], in1=t0_, op=ALU.mult),
                     reads=[B_ug, bt0], writes=[B_ug])

            for n in range(KT):
                (pya, bya), = fm_tiles("w_oa", [n], 8, ya, B_ya)
                (pyb, byb), = fm_tiles("w_ob", [n], 8, cv, B_cv)
                (pyc, byc), = fm_tiles("w_oc", [n], 8, ug, B_ug)
                acc, bacc = tA[1]
                for bi, (py, bpy) in enumerate([(pya, bya), (pyb, byb), (pyc, byc)]):
                    (pgt, bpg), = fm_tiles("w_gate", [bi * 16 + n], KT, hT, B_hT)
                    (t0_, bt0) = tA[0]
                    T.op("act", lambda e, pgt=pgt, t0_=t0_, bi=bi, n=n: e.activation(
                        out=t0_, in_=pgt, func=AF.Sigmoid, bias=ppv(l, "b_gate", bi * 16 + n), scale=1.0),
                        reads=[bpg, B_pp], writes=[bt0])
                    if bi == 0:
                        T.op("dve", lambda e, py=py, t0_=t0_: e.tensor_tensor(out=acc, in0=py, in1=t0_, op=ALU.mult),
                             reads=[bpy, bt0], writes=[bacc])
                    else:
                        T.op("dve", lambda e, py=py, t0_=t0_: e.tensor_tensor(out=t0_, in0=py, in1=t0_, op=ALU.mult),
                             reads=[bpy, bt0], writes=[bt0])
                        if bi == 1:
                            T.op("dve", lambda e, t0_=t0_: e.tensor_tensor(out=acc, in0=acc, in1=t0_, op=ALU.add),
                                 reads=[bacc, bt0], writes=[bacc])
                        else:
                            T.op("dve", lambda e, t0_=t0_, n=n: e.tensor_tensor(
                                out=merged[:, n, :], in0=acc, in1=t0_, op=ALU.add),
                                reads=[bacc, bt0], writes=[B_mg])

            if cfg.dbg and ch == 0 and l == 0:
                s_d = nsem("k4")
                dma("sp", dbg_out[0].rearrange("(kt p) t -> p kt t", p=128), merged, [B_mg], [B_dbg], s_d)
                dma("sp", dbg_out[1].rearrange("(kt p) t -> p kt t", p=128), hT, [B_hT], [B_dbg], s_d)
                dma("sp", dbg_out[2, 0:1024].rearrange("(kt p) t -> p kt t", p=128), ya, [B_ya], [B_dbg], s_d)
                dma("sp", dbg_out[2, 1024:2048].rearrange("(kt p) t -> p kt t", p=128), cv, [B_cv], [B_dbg], s_d)
                dma("sp", dbg_out[3, 0:1024].rearrange("(kt p) t -> p kt t", p=128), ug, [B_ug], [B_dbg], s_d)
            for tt in range(4):
                r0 = t0 + tt * 128
                dma("act", xt, xsrc[r0:r0 + 128, :], [B_xsrc], [B_xt], s_x)
                pts = [next_ps() for _ in range(4)]
                for cb in range(8):
                    w, bw = wload("w_o", KT, cb * 256, 256)
                    pt, pb = pts[cb // 2]
                    for kt in range(KT):
                        T.op("pe", lambda e, pt=pt, w=w, kt=kt, cb=cb, tt=tt: e.matmul(
                            pt[:, (cb % 2) * 256:(cb % 2) * 256 + 256], lhsT=merged[:, kt, tt * 128:(tt + 1) * 128],
                            rhs=w[:, kt, 0:256], start=(kt == 0), stop=(kt == KT - 1)),
                            reads=[bw, B_mg], writes=[pb])
                for q in range(4):
                    pt, pb = pts[q]
                    T.op("dve", lambda e, pt=pt, q=q: e.tensor_tensor(
                        out=rs[:, q * 512:(q + 1) * 512], in0=pt, in1=g1row[:, q * 512:(q + 1) * 512], op=ALU.mult),
                        reads=[pb, B_g1], writes=[B_rs])
                T.op("dve", lambda e: e.scalar_tensor_tensor(out=rs, in0=xt, scalar=cfg.alpha, in1=rs,
                                                             op0=ALU.mult, op1=ALU.add),
                     reads=[B_xt, B_rs], writes=[B_rs])
                layer_norm_rows(rs, B_rs, lnr[:, 0, :], lnr[:, 1, :], B_lnr, st, B_st)
                dma("act", xdst[r0:r0 + 128, :], rs, [B_rs], [B_xdst], s_o)
        T.barrier()

    def moe(l, xsrc, B_xsrc, xdst, B_xdst):
        s_p, s_x, s_o, s_sc = nsem("k5"), nsem("k6"), nsem("k7"), nsem("k8")
        apos[0] = persist_end
        xt, B_xt = carve([D], F32, "xt")
        wr, B_wr = carve([KT, NE], BF16, "wr")
        brr, B_brr = carve([NE], F32, "brr")
        h2T, B_h2T = carve([KT, 128], BF16, "h2T")
        h2tok = [carve([D], BF16, "h2tok%d" % i) for i in range(2)]
        base, B_base = carve([NE], F32, "base")
        sm = [carve([NE], F32, "sm%d" % i) for i in range(6)]
        maskb, B_maskb = carve([NE], BF16, "maskb")
        m8, B_m8 = carve([8], F32, "m8")
        e4, B_e4 = carve([8], F32, "e4")
        destf, B_destf = carve([4], F32, "destf")
        dma("pool", wr, wr_in[l], [], [B_wr], s_p)
        dma("sp", brr, br_in[l:l + 1, :].partition_broadcast(128), [], [B_brr], s_p)
        T.op("dve", lambda e: e.memset(base, 0.0), writes=[B_base])

        for tt in range(NT):
            bl = (tt * 128) // S
            r0 = tt * 128
            dma("act", xt, xsrc[r0:r0 + 128, :], [B_xsrc], [B_xt], s_x)
            for q in range(4):
                pt, pb = next_ps()
                for i in range(4):
                    kt = q * 4 + i
                    T.op("pe", lambda e, pt=pt, i=i, kt=kt: e.matmul(
                        pt[:, i * 128:(i + 1) * 128], lhsT=xt[:, kt * 128:(kt + 1) * 128], rhs=ident_f,
                        start=True, stop=True), reads=[B_xt, B_identf], writes=[pb])
                for i in range(4):
                    kt = q * 4 + i
                    T.op("act", lambda e, pt=pt, i=i, kt=kt, bl=bl: e.activation(
                        out=h2T[:, kt, :], in_=pt[:, i * 128:(i + 1) * 128],
                        func=AF.Identity, bias=mcol(l, bl, 3, kt), scale=mcol(l, bl, 4, kt)),
                        reads=[pb, B_modc], writes=[B_h2T])
            pl, bpl = next_ps()
            for kt in range(KT):
                T.op("pe", lambda e, kt=kt, pl=pl: e.matmul(pl[:, 0:NE], lhsT=h2T[:, kt, :], rhs=wr[:, kt, :],
                                                            start=(kt == 0), stop=(kt == KT - 1)),
                     reads=[B_h2T, B_wr], writes=[bpl])
            (h2t, bh2t) = h2tok[tt % 2]
            for q in range(4):
                pt, pb = next_ps()
                for i in range(4):
                    kt = q * 4 + i
                    T.op("pe", lambda e, pt=pt, i=i, kt=kt: e.matmul(
                        pt[:, i * 128:(i + 1) * 128], lhsT=h2T[:, kt, :], rhs=ident_b, start=True, stop=True),
                        reads=[B_h2T, B_identb], writes=[pb])
                T.op("act", lambda e, pt=pt, q=q, h2t=h2t: e.copy(out=h2t[:, q * 512:(q + 1) * 512], in_=pt),
                     reads=[pb], writes=[bh2t])
            (lg, blg), (oh, boh), (dd, bdd), (t5, bt5), (ov, bov), (cs_, bcs) = sm
            T.op("dve", lambda e, pl=pl: e.tensor_tensor(out=lg, in0=pl[:, 0:NE], in1=brr, op=ALU.add),
                 reads=[bpl, B_brr], writes=[blg])
            T.op("dve", lambda e: e.max(out=m8, in_=lg), reads=[blg], writes=[B_m8])
            T.op("dve", lambda e: e.tensor_scalar(out=e4[:, 4:5], in0=m8[:, 0:1], scalar1=-1.0, scalar2=None, op0=ALU.mult),
                 reads=[B_m8], writes=[B_e4])
            T.op("act", lambda e: e.activation(out=e4[:, 0:4], in_=m8[:, 0:4], func=AF.Exp, bias=e4[:, 4:5], scale=1.0),
                 reads=[B_m8, B_e4], writes=[B_e4])
            T.op("dve", lambda e: e.reduce_sum(out=e4[:, 5:6], in_=e4[:, 0:4], axis=AX.X), reads=[B_e4], writes=[B_e4])
            T.op("dve", lambda e: e.reciprocal(out=e4[:, 5:6], in_=e4[:, 5:6]), reads=[B_e4], writes=[B_e4])
            T.op("dve", lambda e, tt=tt: e.tensor_scalar(out=rw[:, tt, :], in0=e4[:, 0:4], scalar1=e4[:, 5:6], scalar2=None,
                                                         op0=ALU.mult), reads=[B_e4], writes=[B_rw])
            T.op("dve", lambda e: e.tensor_scalar(out=maskb, in0=lg, scalar1=m8[:, 3:4], scalar2=None, op0=ALU.is_ge),
                 reads=[blg, B_m8], writes=[B_maskb])
            pp_, bpp = next_ps()
            T.op("pe", lambda e, pp_=pp_: e.matmul(pp_[:, 0:NE], lhsT=triu_b, rhs=maskb, start=True, stop=True),
                 reads=[B_triu, B_maskb], writes=[bpp])
            T.op("pe", lambda e, pp_=pp_: e.matmul(pp_[:, 64:64 + NE], lhsT=ones_b, rhs=maskb, start=True, stop=True),
                 reads=[B_onesb, B_maskb], writes=[bpp])
            T.op("dve", lambda e, pp_=pp_: e.tensor_tensor(out=dd, in0=pp_[:, 0:NE], in1=base, op=ALU.add),
                 reads=[bpp, B_base], writes=[bdd])
            T.op("dve", lambda e, pp_=pp_: e.tensor_tensor(out=base, in0=pp_[:, 64:64 + NE], in1=base, op=ALU.add),
                 reads=[bpp, B_base], writes=[B_base])
            T.op("dve", lambda e: e.tensor_scalar(out=ov, in0=dd, scalar1=float(CAP), scalar2=1.0e7,
                                                  op0=ALU.is_ge, op1=ALU.mult), reads=[bdd], writes=[bov])
            T.op("dve", lambda e: e.tensor_tensor(out=dd, in0=dd, in1=erow, op=ALU.add), reads=[bdd, B_erow], writes=[bdd])
            T.op("dve", lambda e: e.tensor_tensor(out=dd, in0=dd, in1=ov, op=ALU.add), reads=[bdd, bov], writes=[bdd])
            T.op("dve", lambda e: e.tensor_scalar(out=dd, in0=dd, scalar1=float(NROWS), scalar2=None, op0=ALU.min),
                 reads=[bdd], writes=[bdd])
            for k in range(4):
                T.op("dve", lambda e, k=k: e.tensor_scalar(out=oh, in0=lg, scalar1=m8[:, k:k + 1], scalar2=None,
                                                           op0=ALU.is_equal), reads=[blg, B_m8], writes=[boh])
                T.op("dve", lambda e: e.tensor_tensor(out=oh, in0=oh, in1=dd, op=ALU.mult), reads=[boh, bdd], writes=[boh])
                T.op("dve", lambda e, k=k: e.reduce_sum(out=destf[:, k:k + 1], in_=oh, axis=AX.X),
                     reads=[boh], writes=[B_destf])
            T.op("dve", lambda e, tt=tt: e.tensor_copy(out=ridx[:, tt, :], in_=destf), reads=[B_destf], writes=[B_ridx])
            T.op("dve", lambda e: e.tensor_scalar(out=e4[:, 0:4], in0=destf, scalar1=float(NROWS), scalar2=None,
                                                  op0=ALU.is_lt), reads=[B_destf], writes=[B_e4])
            T.op("dve", lambda e, tt=tt: e.tensor_tensor(out=rw[:, tt, :], in0=rw[:, tt, :], in1=e4[:, 0:4], op=ALU.mult),
                 reads=[B_e4, B_rw], writes=[B_rw])
            for k in range(4):
                T.op("pool", lambda e, k=k, tt=tt, h2t=h2t: e.indirect_dma_start(
                    out=xb, out_offset=bass.IndirectOffsetOnAxis(ap=ridx[:, tt, k:k + 1], axis=0),
                    in_=h2t, in_offset=None),
                    reads=[bh2t, B_ridx], writes=[B_xb], dsem=s_sc)
        if cfg.cnt:
            s_c = nsem("k9")
            dma("sp", cnt_out[l:l + 1, :], base[0:1, :], [B_base], [B_cnt], s_c)

        T.barrier()
        apos[0] = persist_end
        NST = CAP // 128
        bdn, B_bdn = carve([D], BF16, "bdn")
        xe = [carve([D], BF16, "xe%d" % i) for i in range(2)]
        xeT, B_xeT = carve([KT, CAP], BF16, "xeT")
        actT, B_actT = carve([6, CAP], BF16, "actT")
        wg = [carve([KT, 512], BF16, "wg%d" % i) for i in range(3)]
        wd = [carve([6, 1024], BF16, "wd%d" % i) for i in range(2)]
        yo = [carve([D], F32, "yo%d" % i) for i in range(2)]
        te = [carve([512], F32, "te%d" % i) for i in range(3)]
        s_xe, s_wg, s_wd, s_yo = nsem("k10"), [nsem("wg%d" % _i) for _i in range(3)], [nsem("wd%d" % _i) for _i in range(2)], nsem("k11")
        dma("pool", bdn[0:NE, :], bdn_in[l], [], [B_bdn], s_p)
        gq, dq_ = [0], [0]
        segs = []
        o_ = 0
        while o_ < CAP:
            segs.append((o_, min(512, CAP - o_)))
            o_ += 512
        for ex in range(NE):
            for s_ in range(NST):
                (xe_, bxe) = xe[s_ % 2]
                r0 = ex * CAP + s_ * 128
                dma("act", xe_, xb[r0:r0 + 128, :], [B_xb], [bxe], s_xe)
                for q in range(4):
                    pt, pb = next_ps()
                    for i in range(4):
                        kt = q * 4 + i
                        T.op("pe", lambda e, pt=pt, i=i, kt=kt, xe_=xe_: e.matmul(
                            pt[:, i * 128:(i + 1) * 128], lhsT=xe_[:, kt * 128:(kt + 1) * 128], rhs=ident_b,
                            start=True, stop=True), reads=[bxe, B_identb], writes=[pb])
                    T.op("act", lambda e, pt=pt, q=q, s_=s_: e.copy(
                        out=xeT[:, q * 4:(q + 1) * 4, s_ * 128:(s_ + 1) * 128],
                        in_=pt.rearrange("p (a b) -> p a b", a=4)), reads=[pb], writes=[B_xeT])
            for grp in range(3):
                i = gq[0] % 3
                gq[0] += 1
                (w, bw) = wg[i]
                wsrc = wfull[("w_gu", l)].ap()
                for half, c0 in enumerate([grp * 256, DE + grp * 256]):
                    src = wsrc[ex * D:(ex + 1) * D, c0:c0 + 256].rearrange("(kt p) c -> p kt c", p=128)
                    dma("sp", w[:, :, half * 256:(half + 1) * 256], src, [B_wfull[("w_gu", l)]], [bw], s_wg[i])
                for jj in range(2):
                    gt = grp * 2 + jj
                    bg_ = ppv(l, "b_gu", ex * 12 + gt)
                    bu_ = ppv(l, "b_gu", ex * 12 + 6 + gt)
                    for (c0, cn) in segs:
                        pg, bpg = next_ps()
                        pu, bpu = next_ps()
                        for which, (pt, pb) in ((0, (pg, bpg)), (1, (pu, bpu))):
                            for kt in range(KT):
                                T.op("pe", lambda e, pt=pt, kt=kt, w=w, which=which, jj=jj, c0=c0, cn=cn: e.matmul(
                                    pt[:, 0:cn], lhsT=w[:, kt, which * 256 + jj * 128: which * 256 + (jj + 1) * 128],
                                    rhs=xeT[:, kt, c0:c0 + cn], start=(kt == 0), stop=(kt == KT - 1)),
                                    reads=[bw, B_xeT], writes=[pb])
                        (gc, bgc), (sg, bsg), (uc, buc) = te
                        T.op("dve", lambda e, pg=pg, cn=cn, bg_=bg_, gc=gc: e.tensor_scalar(
                            out=gc[:, 0:cn], in0=pg[:, 0:cn], scalar1=bg_, scalar2=7.0, op0=ALU.add, op1=ALU.min),
                            reads=[bpg, B_pp], writes=[bgc])
                        T.op("dve", lambda e, pu=pu, cn=cn, bu_=bu_, uc=uc: e.tensor_scalar(
                            out=uc[:, 0:cn], in0=pu[:, 0:cn], scalar1=bu_, scalar2=7.0, op0=ALU.add, op1=ALU.min),
                            reads=[bpu, B_pp], writes=[buc])
                        T.op("act", lambda e, cn=cn, gc=gc, sg=sg: e.activation(out=sg[:, 0:cn], in_=gc[:, 0:cn], func=AF.Sigmoid, scale=1.702),
                             reads=[bgc], writes=[bsg])
                        T.op("dve", lambda e, cn=cn, uc=uc: e.tensor_scalar(out=uc[:, 0:cn], in0=uc[:, 0:cn], scalar1=-7.0, scalar2=1.0,
                                                                    op0=ALU.max, op1=ALU.add), reads=[buc], writes=[buc])
                        T.op("dve", lambda e, cn=cn, gc=gc, sg=sg: e.tensor_tensor(out=gc[:, 0:cn], in0=gc[:, 0:cn], in1=sg[:, 0:cn], op=ALU.mult),
                             reads=[bgc, bsg], writes=[bgc])
                        T.op("dve", lambda e, gt=gt, c0=c0, cn=cn, gc=gc, uc=uc: e.tensor_tensor(
                            out=actT[:, gt, c0:c0 + cn], in0=gc[:, 0:cn], in1=uc[:, 0:cn], op=ALU.mult),
                            reads=[bgc, buc], writes=[B_actT])
            T.op("dve", lambda e, ex=ex: e.tensor_scalar(out=selt[0:NE, :], in0=ones_b[0:NE, :], scalar1=ident_f[0:NE, ex:ex + 1],
                                                         scalar2=None, op0=ALU.mult), reads=[B_onesb, B_identf], writes=[B_selt])
            wds = []
            for half in range(2):
                i = dq_[0] % 2
                dq_[0] += 1
                (w, bw) = wd[i]
                src = wfull[("w_dn", l)].ap()[ex * DE:(ex + 1) * DE, half * 1024:(half + 1) * 1024].rearrange(
                    "(kt p) c -> p kt c", p=128)
                dma("sp", w, src, [B_wfull[("w_dn", l)]], [bw], s_wd[i])
                wds.append((w, bw))
            for s_ in range(NST):
                (yo_, byo) = yo[s_ % 2]
                for q in range(4):
                    (w, bw) = wds[q // 2]
                    pt, pb = next_ps()
                    for kt in range(6):
                        T.op("pe", lambda e, pt=pt, kt=kt, w=w, q=q, s_=s_: e.matmul(
                            pt, lhsT=actT[:, kt, s_ * 128:(s_ + 1) * 128], rhs=w[:, kt, (q % 2) * 512:(q % 2) * 512 + 512],
                            start=(kt == 0), stop=False), reads=[bw, B_actT], writes=[pb])
                    T.op("pe", lambda e, pt=pt, q=q: e.matmul(
                        pt, lhsT=selt[0:NE, :], rhs=bdn[0:NE, q * 512:(q + 1) * 512],
                        start=False, stop=True), reads=[B_selt, B_bdn], writes=[pb])
                    T.op("act", lambda e, pt=pt, q=q, yo_=yo_: e.copy(out=yo_[:, q * 512:(q + 1) * 512], in_=pt),
                         reads=[pb], writes=[byo])
                r0 = ex * CAP + s_ * 128
                for hh in range(2):
                    dma("act", ybh[hh][r0:r0 + 128, :], yo_[:, hh * 1024:(hh + 1) * 1024], [byo], [B_yb], s_yo)

        T.barrier()
        apos[0] = persist_end
        xt, B_xt = carve([D], F32, "xt")
        rs, B_rs = carve([D], F32, "rs")
        g2row, B_g2 = carve([D], F32, "g2row")
        lnr, B_lnr = carve([2, D], F32, "lnr")
        tmp128, B_tmp128 = carve([128], F32, "tmp128")
        st, B_st = carve([32], F32, "st")
        gb = [carve([D], F32, "gb%d" % i) for i in range(4)]
        s_g = [nsem("sg%d" % _i) for _i in range(4)]
        dma("sp", lnr[:, 0, :], rows_in[l, 2:3, :].partition_broadcast(128), [], [B_lnr], s_p)
        dma("sp", lnr[:, 1, :], rows_in[l, 3:4, :].partition_broadcast(128), [], [B_lnr], s_p)
        for tt in range(NT):
            bl = (tt * 128) // S
            r0 = tt * 128
            if (tt * 128) % S == 0:
                grow_build(l, bl, 5, g2row, B_g2, tmp128, B_tmp128)
            dma("act", xt, xsrc[r0:r0 + 128, :], [B_xsrc], [B_xt], s_x)
            for k in range(4):
                for hh in range(2):
                    T.op("pool", lambda e, k=k, tt=tt, hh=hh: e.indirect_dma_start(
                        out=gb[k][0][:, hh * 1024:(hh + 1) * 1024], out_offset=None, in_=ybh[hh],
                        in_offset=bass.IndirectOffsetOnAxis(ap=ridx[:, tt, k:k + 1], axis=0)),
                        reads=[B_yb, B_ridx], writes=[gb[k][1]], dsem=s_g[k])
            T.op("dve", lambda e, tt=tt: e.tensor_scalar(out=rs, in0=gb[0][0], scalar1=rw[:, tt, 0:1], scalar2=None,
                                                         op0=ALU.mult), reads=[gb[0][1], B_rw], writes=[B_rs])
            for k in range(1, 4):
                T.op("dve", lambda e, k=k, tt=tt: e.scalar_tensor_tensor(
                    out=rs, in0=gb[k][0], scalar=rw[:, tt, k:k + 1], in1=rs, op0=ALU.mult, op1=ALU.add),
                    reads=[gb[k][1], B_rw, B_rs], writes=[B_rs])
            T.op("pool", lambda e: e.tensor_tensor(out=rs, in0=rs, in1=g2row, op=ALU.mult), reads=[B_rs, B_g2], writes=[B_rs])
            T.op("dve", lambda e: e.scalar_tensor_tensor(out=rs, in0=xt, scalar=cfg.alpha, in1=rs, op0=ALU.mult, op1=ALU.add),
                 reads=[B_xt, B_rs], writes=[B_rs])
            layer_norm_rows(rs, B_rs, lnr[:, 0, :], lnr[:, 1, :], B_lnr, st, B_st)
            dma("act", xdst[r0:r0 + 128, :], rs, [B_rs], [B_xdst], s_o)
        T.barrier()

    B_xin = Buf("xin")
    for l in range(L):
        src, bsrc = (x_in, B_xin) if l == 0 else (xres, B_xres)
        mixer(l, src, bsrc, xres, B_xres)
        if cfg.dbg and cfg.dbg == "mixer":
            break
        dst, bdst = (y_out, B_y) if l == L - 1 else (xres, B_xres)
        moe(l, xres, B_xres, dst, bdst)
    if cfg.dbg == "mixer":
        apos[0] = persist_end
        xt, B_xt = carve([D], F32, "xt")
        s1, s2 = T.dma_sem(), T.dma_sem()
        for tt in range(NT):
            dma("sp", xt, xres[tt * 128:(tt + 1) * 128, :], [B_xres], [B_xt], s1)
            dma("sp", y_out[tt * 128:(tt + 1) * 128, :], xt, [B_xt], [B_y], s2)
    T.emit()
    es.close()
    return nc


def _pcol(v):
    v = np.asarray(v, np.float32)
    sh = v.shape
    v = v.reshape(sh[:-1] + (sh[-1] // 128, 128))
    return np.moveaxis(v, -1, 0)


def make_in_maps(cfg, inp):
    L, NB, S = cfg.L, cfg.NB, cfg.S
    f = lambda a: np.ascontiguousarray(np.asarray(a, np.float32))
    ident = np.eye(128, dtype=np.float32)
    triu = np.triu(np.ones((128, 128), np.float32), 1)
    sel = np.zeros((NE, NE * 128), np.float32)
    for e in range(NE):
        sel[e, e * 128:(e + 1) * 128] = 1.0
    erow = (np.arange(NE, dtype=np.float32) * cfg.CAP)[None, :]
    adaT = f(_pcol(inp["ada_table"][:L]).reshape(128, -1))
    pp = np.zeros((128, L, PPW), np.float32)
    for l in range(L):
        def put(name, arr):
            o, w = PP[name]
            assert arr.shape == (128, w), (name, arr.shape)
            pp[:, l, o:o + w] = arr
        put("conv_a", _pcol(inp["conv_a"][l]).transpose(0, 2, 1).reshape(128, -1))
        put("dw_w", _pcol(inp["dw_b_w"][l]).transpose(0, 2, 1).reshape(128, -1))
        put("dw_b", _pcol(inp["dw_b_b"][l]))
        put("lnb_g", _pcol(inp["ln_b_g"][l]))
        put("lnb_b", _pcol(inp["ln_b_b"][l]))
        put("b_gate", _pcol(inp["b_gate"][l]))
        put("b_gu", _pcol(inp["b_gu"][l]).reshape(128, -1))
    pp = f(pp.reshape(128, -1))
    rows = np.zeros((L, 6, D), np.float32)
    for l in range(L):
        rows[l, 0], rows[l, 1] = inp["ln1_g"][l], inp["ln1_b"][l]
        rows[l, 2], rows[l, 3] = inp["ln2_g"][l], inp["ln2_b"][l]
        rows[l, 4, :WA], rows[l, 4, WA:] = inp["ln_c_g"][l], inp["ln_c_b"][l]
    wr = f(np.stack([_pcol(np.asarray(inp["w_router"][l]).T).transpose(0, 2, 1) for l in range(L)]))
    x = np.asarray(inp["x"], np.float32)
    c = np.asarray(inp["c"], np.float32)
    maps = []
    for r in range(NCORES):
        m = {}
        m["x"] = f(x[r * NB:(r + 1) * NB, :S].reshape(NB * S, D))
        m["cT"] = f(_pcol(c).transpose(0, 2, 1))
        bs = np.zeros((128, NB, 16), np.float32)
        for bl in range(NB):
            bs[:, bl, r * NB + bl] = 1.0
        m["bsel"] = bs.reshape(128, NB * 16)
        m["w_ada"] = f(np.asarray(inp["w_ada"])[:, r * 1536:(r + 1) * 1536].reshape(KT, 128, 1536).transpose(1, 0, 2))
        m["b_ada"] = f(_pcol(np.asarray(inp["b_ada"])[r * 1536:(r + 1) * 1536]))
        m["adaT"] = adaT
        m["w_in_s"] = f(inp["w_in"][:L, r * 256:(r + 1) * 256])
        m["w_gate_s"] = f(inp["w_gate"][:L, r * 256:(r + 1) * 256])
        m["w_o_s"] = f(inp["w_o"][:L, r * 256:(r + 1) * 256])
        m["w_oa_s"] = f(inp["w_out_a"][:L, r * 128:(r + 1) * 128])
        m["w_ob_s"] = f(inp["w_out_b"][:L, r * 128:(r + 1) * 128])
        m["w_oc_s"] = f(inp["w_out_c"][:L, r * 128:(r + 1) * 128])
        m["w_gu_s"] = f(np.asarray(inp["w_gu"][:L, r * 4:(r + 1) * 4]).reshape(L, 4 * D, 2 * DE))
        m["w_dn_s"] = f(np.asarray(inp["w_down"][:L, r * 4:(r + 1) * 4]).reshape(L, 4 * DE, D))
        m["pp"] = pp
        m["rows"] = rows
        m["sgu_w"] = f(inp["sgu_w"][:L])
        m["sgu_b"] = f(np.asarray(inp["sgu_b"][:L]).reshape(L, -1))
        m["w_router"] = wr
        m["b_router"] = f(inp["b_router"][:L])
        m["b_down"] = f(inp["b_down"][:L])
        m["ident"], m["triu"], m["sel"], m["erow"] = ident, triu, sel, erow
        maps.append(m)
    return maps


_NC_CACHE = {}


def kernel(**inputs):
    cfg = Cfg()
    if "nc" not in _NC_CACHE:
        _NC_CACHE["nc"] = build(cfg)
    nc = _NC_CACHE["nc"]
    maps = make_in_maps(cfg, inputs)
    res = run_bass_kernel_spmd(nc, maps, core_ids=list(range(NCORES)))
    out = np.stack([r["y"].reshape(cfg.NB, cfg.S, D) for r in res.results], axis=0)
    return out.reshape(16, cfg.S, D).astype(np.float32)
```
